# Optimizing a Trainium2 kernel written in Bass

```python
import jax, jax.numpy as jnp
from jax import lax
import numpy as np

D_MODEL = 1024
BATCH = 4
SEQ = 4096
DEPTH = 2

CTX_LEN = 256
GRID_W = 64
POS_BASE = 10000.0
D_FOURIER = D_MODEL // 2
FOURIER_GROUPS = 4
D_LRU = D_MODEL // 2
LRU_HEADS = 8
LRU_HEAD_DIM = D_LRU // LRU_HEADS
CONV_W = 4
LRU_C = 8.0
D_IN = D_FOURIER + 2 * D_LRU
D_MIX = D_FOURIER + D_LRU
N_EXPERTS = 16
EC_CAPACITY = 2
D_FF = 2816
N_MOD = 6
EPS = 1e-6

kernel_name = "hybrid_fourier_rglru_ec_moe_dit"


def rmsnorm(x, g):
    xf = x.astype(jnp.float32)
    y = xf * lax.rsqrt(jnp.mean(xf * xf, axis=-1, keepdims=True) + EPS) * g.astype(jnp.float32)
    return y.astype(x.dtype)


def modulate(h, shift, scale):
    return h * (1 + scale) + shift


def grid_pos_embed(rows, d):
    rr, cc = jnp.meshgrid(jnp.arange(rows, dtype=jnp.float32), jnp.arange(GRID_W, dtype=jnp.float32), indexing="ij")
    quarter = d // 4
    omega = 1.0 / (POS_BASE ** (jnp.arange(quarter, dtype=jnp.float32) / quarter))

    def enc(p):
        ang = p.reshape(-1)[:, None] * omega
        return jnp.concatenate([jnp.sin(ang), jnp.cos(ang)], axis=-1)

    return jnp.concatenate([enc(rr), enc(cc)], axis=-1)


def fourier_mix(u):
    b, n, _ = u.shape
    ug = u.astype(jnp.float32).reshape(b, n, FOURIER_GROUPS, D_FOURIER // FOURIER_GROUPS)
    y = jnp.fft.fft2(ug, axes=(1, 3), norm="ortho").real
    return y.reshape(b, n, D_FOURIER).astype(u.dtype)


def centred_dwconv(u, w, bias):
    n = u.shape[1]
    left = (CONV_W - 1) // 2
    up = jnp.pad(u, ((0, 0), (left, CONV_W - 1 - left), (0, 0)))
    y = up[:, 0:n] * w[0]
    for k in range(1, CONV_W):
        y = y + up[:, k:k + n] * w[k]
    return y + bias


def _combine(left, right):
    a_l, b_l = left
    a_r, b_r = right
    return a_l * a_r, a_r * b_l + b_r


def linear_scan(a, b, h0, reverse):
    if reverse:
        a = jnp.flip(a, axis=1)
        b = jnp.flip(b, axis=1)
    if h0 is not None:
        b = b.at[:, 0].add(a[:, 0] * h0)
    _, h = lax.associative_scan(_combine, (a, b), axis=1)
    return jnp.flip(h, axis=1) if reverse else h


def bidir_rglru(u, conv_w, conv_b, wa, ba, wi, bi, lam, h0):
    xc = centred_dwconv(u, conv_w, conv_b).astype(jnp.float32)
    bsz, n, _ = xc.shape
    xh = xc.reshape(bsz, n, LRU_HEADS, LRU_HEAD_DIM)
    y = jnp.zeros_like(xc)
    finals = []
    for dirn in range(2):
        r = jax.nn.sigmoid(jnp.einsum("bnhi,hij->bnhj", xh, wa[dirn].astype(jnp.float32)).reshape(bsz, n, D_LRU)
                           + ba[dirn].astype(jnp.float32))
        gi = jax.nn.sigmoid(jnp.einsum("bnhi,hij->bnhj", xh, wi[dirn].astype(jnp.float32)).reshape(bsz, n, D_LRU)
                            + bi[dirn].astype(jnp.float32))
        log_a = -LRU_C * r * jax.nn.softplus(-lam[dirn].astype(jnp.float32))
        a = jnp.exp(log_a)
        drive = jnp.sqrt(-jnp.expm1(2.0 * log_a)) * (gi * xc)
        init = None if h0 is None else h0[dirn]
        h = linear_scan(a, drive, init, reverse=(dirn == 1))
        y = y + h
        finals.append(h[:, -1] if dirn == 0 else h[:, 0])
    return y, (finals[0], finals[1])


def token_mixer(h, w_in, conv_w, conv_b, wa, ba, wi, bi, lam, w_out, h0):
    proj = h @ w_in
    u_f = proj[..., :D_FOURIER]
    u_x = proj[..., D_FOURIER:D_FOURIER + D_LRU]
    u_g = proj[..., D_FOURIER + D_LRU:]
    y_f = fourier_mix(u_f)
    y_r, finals = bidir_rglru(u_x, conv_w, conv_b, wa, ba, wi, bi, lam, h0)
    y_r = y_r.astype(h.dtype) * jax.nn.gelu(u_g)
    return jnp.concatenate([y_f, y_r], axis=-1) @ w_out, finals


def expert_choice_ffn(h, w_router, w_gate, w_up, w_down):
    b, n, _ = h.shape
    cap = EC_CAPACITY * n // N_EXPERTS
    aff = jax.nn.softmax(jnp.einsum("bnd,de->bne", h.astype(jnp.float32), w_router.astype(jnp.float32)), axis=-1)
    g, idx = lax.top_k(jnp.swapaxes(aff, 1, 2), cap)
    bidx = jnp.arange(b)[:, None, None]
    xg = h[bidx, idx]
    hid = jax.nn.silu(jnp.einsum("becd,edf->becf", xg, w_gate)) * jnp.einsum("becd,edf->becf", xg, w_up)
    y = jnp.einsum("becf,efd->becd", hid, w_down) * g[..., None].astype(h.dtype)
    return jnp.zeros_like(h).at[bidx, idx].add(y)


def setup_inputs(seed: int = 0) -> dict:
    key = jax.random.key(seed)
    k = jax.random.split(key, 24)
    f32 = jnp.float32
    nrm = lambda kk, shape, s: jax.random.normal(kk, shape, f32) * s
    u = jax.random.uniform(k[16], (DEPTH, 2, D_LRU), f32, minval=0.9, maxval=0.999)
    a0 = u ** (1.0 / LRU_C)
    return {
        "x": nrm(k[0], (BATCH, SEQ, D_MODEL), 1.0),
        "c": nrm(k[1], (BATCH, D_MODEL), 1.0),
        "ctx": nrm(k[2], (BATCH, CTX_LEN, D_MODEL), 1.0),
        "c_ctx": nrm(k[3], (D_MODEL,), 1.0),
        "w_mod": nrm(k[4], (DEPTH, D_MODEL, N_MOD * D_MODEL), 0.5 * D_MODEL ** -0.5),
        "b_mod": nrm(k[5], (DEPTH, N_MOD * D_MODEL), 0.02),
        "norm1_g": 1.0 + nrm(k[6], (DEPTH, D_MODEL), 0.02),
        "norm2_g": 1.0 + nrm(k[7], (DEPTH, D_MODEL), 0.02),
        "w_in": nrm(k[8], (DEPTH, D_MODEL, D_IN), D_MODEL ** -0.5),
        "conv_w": nrm(k[9], (DEPTH, CONV_W, D_LRU), CONV_W ** -0.5),
        "conv_b": nrm(k[10], (DEPTH, D_LRU), 0.02),
        "lru_wa": nrm(k[11], (DEPTH, 2, LRU_HEADS, LRU_HEAD_DIM, LRU_HEAD_DIM), LRU_HEAD_DIM ** -0.5),
        "lru_ba": nrm(k[12], (DEPTH, 2, D_LRU), 0.02),
        "lru_wi": nrm(k[13], (DEPTH, 2, LRU_HEADS, LRU_HEAD_DIM, LRU_HEAD_DIM), LRU_HEAD_DIM ** -0.5),
        "lru_bi": nrm(k[14], (DEPTH, 2, D_LRU), 0.02),
        "lru_lambda": jnp.log(a0) - jnp.log1p(-a0),
        "w_out": nrm(k[15], (DEPTH, D_MIX, D_MODEL), D_MIX ** -0.5),
        "w_router": nrm(k[17], (DEPTH, D_MODEL, N_EXPERTS), D_MODEL ** -0.5),
        "w_gate": nrm(k[18], (DEPTH, N_EXPERTS, D_MODEL, D_FF), D_MODEL ** -0.5),
        "w_up": nrm(k[19], (DEPTH, N_EXPERTS, D_MODEL, D_FF), D_MODEL ** -0.5),
        "w_down": nrm(k[20], (DEPTH, N_EXPERTS, D_FF, D_MODEL), D_FF ** -0.5),
        "final_g": 1.0 + nrm(k[21], (D_MODEL,), 0.02),
    }


def reference(x, c, ctx, c_ctx, w_mod, b_mod, norm1_g, norm2_g, w_in, conv_w, conv_b,
              lru_wa, lru_ba, lru_wi, lru_bi, lru_lambda, w_out, w_router, w_gate, w_up, w_down, final_g):
    ROWS = x.shape[1] // GRID_W
    x = x + grid_pos_embed(ROWS, x.shape[-1]).astype(x.dtype)[None]
    cond = jax.nn.silu(c)
    cond_ctx = jax.nn.silu(c_ctx)
    for l in range(DEPTH):
        last = l == DEPTH - 1
        mod = cond @ w_mod[l] + b_mod[l]
        sh1, sc1, g1, sh2, sc2, g2 = jnp.split(mod[:, None, :], N_MOD, axis=-1)
        mc = cond_ctx @ w_mod[l] + b_mod[l]
        csh1, csc1, cg1, csh2, csc2, cg2 = jnp.split(mc, N_MOD, axis=-1)
        lru_p = (conv_w[l], conv_b[l], lru_wa[l], lru_ba[l], lru_wi[l], lru_bi[l], lru_lambda[l])

        hc = modulate(rmsnorm(ctx, norm1_g[l]), csh1, csc1)
        if last:
            u_cx = hc @ w_in[l][:, D_FOURIER:D_FOURIER + D_LRU]
            _, ctx_states = bidir_rglru(u_cx, *lru_p, None)
        else:
            ctx_mix, ctx_states = token_mixer(hc, w_in[l], *lru_p, w_out[l], None)
            ctx = ctx + cg1 * ctx_mix
            hc2 = modulate(rmsnorm(ctx, norm2_g[l]), csh2, csc2)
            ctx = ctx + cg2 * expert_choice_ffn(hc2, w_router[l], w_gate[l], w_up[l], w_down[l])

        hx = modulate(rmsnorm(x, norm1_g[l]), sh1, sc1)
        x_mix, _ = token_mixer(hx, w_in[l], *lru_p, w_out[l], ctx_states)
        x = x + g1 * x_mix
        hx2 = modulate(rmsnorm(x, norm2_g[l]), sh2, sc2)
        x = x + g2 * expert_choice_ffn(hx2, w_router[l], w_gate[l], w_up[l], w_down[l])
    return rmsnorm(x, final_g)
```

```python
import numpy as np
from contextlib import ExitStack
import concourse.bass as bass
import concourse.mybir as mybir
from concourse.bass_utils import run_bass_kernel_spmd

F32 = mybir.dt.float32
BF16 = mybir.dt.bfloat16
I32 = mybir.dt.int32
U32 = mybir.dt.uint32
AF = mybir.ActivationFunctionType
ALU = mybir.AluOpType
AX = mybir.AxisListType

D = 1024
DK = 8
NE = 16
NCORES = 8
B = 4
EPS = 1e-6
LRU_C = 8.0


import types


def _snap(fn):
    if fn.__closure__ is None:
        return fn
    cells = tuple(types.CellType(c.cell_contents) for c in fn.__closure__)
    g = types.FunctionType(fn.__code__, fn.__globals__, fn.__name__, fn.__defaults__, cells)
    g.__kwdefaults__ = fn.__kwdefaults__
    return g


class K:
    def __init__(self, nc):
        self.nc = nc
        self.st = ExitStack()
        self.eng = {"pe": nc.tensor, "act": nc.scalar, "dve": nc.vector, "pool": nc.gpsimd, "sp": nc.sync}
        self.esem = {}
        self.ecnt = {}
        for e in ("pe", "act", "dve", "pool"):
            self.esem[e] = self.st.enter_context(nc.semaphore("es_" + e))
            self.ecnt[e] = 0
        self.waited = {e: {} for e in self.eng}
        self.res = {}
        self.dsem = {}
        self.pending = {e: ([], []) for e in self.eng}
        self.prog = {e: [] for e in self.eng}

    def _wait(self, e, evs):
        best = {}
        for ev in evs:
            if ev is None:
                continue
            sem, val, src = ev
            if src == e and e == "pe":
                continue
            nm = id(sem)
            if self.waited[e].get(nm, 0) >= val:
                continue
            if nm not in best or best[nm][1] < val:
                best[nm] = (sem, val)
        for nm, (sem, val) in best.items():
            self.prog[e].append(("w", sem, val))
            self.waited[e][nm] = val

    def _deps(self, r, w):
        deps = []
        for k in r:
            s = self.res.get(k)
            if s and s["w"] is not None:
                deps.append(s["w"])
        for k in w:
            s = self.res.get(k)
            if s:
                if s["w"] is not None:
                    deps.append(s["w"])
                deps.extend(s["r"])
        return deps

    def _record(self, ev, r, w):
        for k in r:
            self.res.setdefault(k, {"w": None, "r": []})["r"].append(ev)
        for k in w:
            self.res[k] = {"w": ev, "r": []}

    def op(self, e, fn, r=(), w=(), sig=True):
        fn = _snap(fn)
        r = list(r); w = list(w)
        self._wait(e, self._deps(r, w))
        if sig:
            self.ecnt[e] += 1
            self.prog[e].append(("i", fn, self.esem[e], 1))
            ev = (self.esem[e], self.ecnt[e], e)
            pr, pw = self.pending[e]
            self._record(ev, r + pr, w + pw)
            self.pending[e] = ([], [])
        else:
            self.prog[e].append(("i", fn, None, 0))
            self.pending[e][0].extend(r)
            self.pending[e][1].extend(w)
            for k in w:
                self.res[k] = {"w": None, "r": []}

    def dma(self, q, fn, r=(), w=(), key=None, inc=16):
        fn = _snap(fn)
        r = list(r); w = list(w)
        if key is None:
            key = ("d",) + tuple(w if w else r)
        if key not in self.dsem:
            self.dsem[key] = [self.st.enter_context(self.nc.semaphore("ds%d" % len(self.dsem))), 0]
        ent = self.dsem[key]
        self._wait(q, self._deps(r, w))
        ent[1] += inc
        self.prog[q].append(("i", fn, ent[0], inc))
        self._record((ent[0], ent[1], "dma"), r, w)

    def barrier(self):
        evs = [(self.esem[e], self.ecnt[e], "x") for e in self.esem if self.ecnt[e] > 0]
        evs += [(s, c, "dma") for (s, c) in self.dsem.values() if c > 0]
        for e in self.eng:
            self._wait(e, evs)

    def flush(self):
        def replay(e):
            def f(eng):
                for a in self.prog[e]:
                    if a[0] == "w":
                        eng.wait_ge(a[1], a[2])
                    else:
                        ins = a[1](eng)
                        if a[2] is not None:
                            ins.then_inc(a[2], a[3])
            return f
        with self.nc.Block() as block:
            block.tensor(replay("pe"))
            block.scalar(replay("act"))
            block.vector(replay("dve"))
            block.gpsimd(replay("pool"))
            block.sync(replay("sp"))
        self.prog = {e: [] for e in self.eng}


class Arena:
    def __init__(self, k):
        self.k = k
        self.st = ExitStack()
        self.n = 0

    def t(self, name, shape, dt):
        Arena_id[0] += 1
        return self.st.enter_context(self.k.nc.sbuf_tensor("%s_%d" % (name, Arena_id[0]), list(shape), dt))

    def end(self):
        self.k.barrier()
        self.k.flush()
        self.st.close()


Arena_id = [0]


def build(cfg):
    T, TC, DFF = cfg["T"], cfg["TC"], cfg["DFF"]
    GRIDW = 64
    CL, CC = 2 * T // NE, 2 * TC // NE
    NFB = DFF // 256
    NTE = B * (CL + CC)
    W = 2 * (CL + CC)
    RT = T + TC
    nc = bass.Bass("TRN2", target_bir_lowering=False)
    k = K(nc)

    def din(name, shape, dt=F32):
        return nc.dram_tensor(name, list(shape), dt, kind="ExternalInput").ap()

    x_in = din("x", [T, D]); ctx_in = din("ctx", [TC, D]); condT = din("condT", [128, DK, 2])
    w_mod = din("w_mod", [2, D, 6 * D]); b_mod = din("b_mod", [2, 6 * D])
    ng = din("ng", [5, D])
    w_in = din("w_in", [2, D, 1536]); w_out = din("w_out", [2, D, D])
    lruv = din("lruv", [2, 512, 8]); lrud = din("lrud", [2, 2, 512, 4]); bd = din("bd", [2, 16, 128, 128])
    w_router = din("w_router", [2, D, NE])
    wg = din("wg", [2, 2, D, DFF]); wu = din("wu", [2, 2, D, DFF]); wd = din("wd", [2, 2, DFF, D])
    sel = din("sel", [NE, 2]); ybase = din("ybase", [128, 2])
    ident = din("ident", [128, 128]); csk = din("csk", [2, 128, 256]); er = din("er", [T // GRIDW, 512]); selh = din("selh", [2, 128]); ec = din("ec", [GRIDW, 512])
    sel2 = din("sel2", [2, 2, 128]); ones1 = din("ones1", [1, 128])
    out = nc.dram_tensor("out", [T, D], F32, kind="ExternalOutput").ap()

    def dscr(name, shape, dt=F32):
        return nc.dram_tensor(name, list(shape), dt).ap()

    xres = dscr("xres", [T, D]); cres = dscr("cres", [TC, D])
    UX = dscr("UX", [512, T]); GG = dscr("GG", [512, T]); MIXT = dscr("MIXT", [D, T], BF16)
    FCL = dscr("FCL", [T, T], BF16); FSL = dscr("FSL", [T, T], BF16)
    FCC = dscr("FCC", [TC, TC], BF16); FSC = dscr("FSC", [TC, TC], BF16)
    MOD = dscr("MOD", [2, 2, 6 * D])
    HX2 = dscr("HX2", [RT, D], BF16); HX2A = dscr("HX2A", [NCORES * RT, D], BF16)
    TAB = dscr("TAB", [NE, W]); TABA = dscr("TABA", [NCORES * NE, W])
    Y = dscr("Y", [2 * NTE, D]); YA = dscr("YA", [NCORES * 2 * NTE, D])

    P = ExitStack()

    def psb(name, shape, dt):
        return P.enter_context(nc.sbuf_tensor(name, list(shape), dt))

    identf = psb("identf", [128, 128], F32); identb = psb("identb", [128, 128], BF16); cskb = psb("cskb", [128, 2, 256], BF16)
    sel_sb = psb("sel_sb", [NE, 2], F32); ybase_sb = psb("ybase_sb", [128, 2], F32)
    sel2_sb = psb("sel2_sb", [2, 2, 128], F32); selh_sb = psb("selh_sb", [2, 128], F32); ones_sb = psb("ones_sb", [1, 128], F32)
    bdb = psb("bdb", [128, 16, 128], BF16); lruv_sb = psb("lruv_sb", [128, 4, 8], F32); lrud_sb = psb("lrud_sb", [128, 2, 4, 4], F32)
    nls_sb = psb("nls_sb", [128, 2, 4], F32)
    states = psb("states", [128, 4, 2], F32)
    wr_sb = psb("wr_sb", [128, DK, NE], F32)
    VAL = {"lat": psb("VALl", [NE, CL], F32), "ctx": psb("VALc", [NE, max(CC, 8)], F32)}
    IDXU = {"lat": psb("IDXUl", [NE, CL], U32), "ctx": psb("IDXUc", [NE, max(CC, 8)], U32)}
    IDXF = {"lat": psb("IDXFl", [NE, CL], F32), "ctx": psb("IDXFc", [NE, max(CC, 8)], F32)}
    OWNI = {"lat": psb("OWNIl", [128, (CL + 127) // 128, NE], I32), "ctx": psb("OWNIc", [128, 1, NE], I32)}
    OWNG = {"lat": psb("OWNGl", [128, (CL + 127) // 128, NE], F32), "ctx": psb("OWNGc", [128, 1, NE], F32)}
    ssv = psb("ssv", [128, 8], F32)
    psT = [P.enter_context(nc.psum_tensor("psT%d" % i, [128, 1024], BF16)) for i in range(2)]
    psF = [P.enter_context(nc.psum_tensor("psF%d" % i, [128, 512], F32)) for i in range(6)]
    rr = {"T": 0, "F": 0}

    def nxT():
        rr["T"] = (rr["T"] + 1) % 2
        return psT[rr["T"]], "psT%d" % rr["T"]

    def nxF():
        rr["F"] = (rr["F"] + 1) % 6
        return psF[rr["F"]], "psF%d" % rr["F"]

    ld = lambda dst, src, key, q="sp", r=(): k.dma(q, lambda e: e.dma_start(out=dst, in_=src), r=r, w=[key])
    ld(identf[:], ident, "identf"); ld(identb[:], ident, "identb", "pool"); ld(cskb[:], csk.rearrange("j p n -> p j n"), "cskb", "pool")
    ld(sel_sb[:], sel, "sel_sb"); ld(ybase_sb[:], ybase, "ybase_sb"); ld(sel2_sb[:], sel2, "sel2_sb"); ld(selh_sb[:], selh, "selh_sb"); ld(ones_sb[:], ones1, "ones_sb")

    A = Arena(k)
    negpi = psb("negpi", [128, 1], F32)
    k.op("dve", lambda e: e.memset(negpi[:], -float(np.pi) * (1 - 1e-6)), w=["negpi"])
    for (TT, FCd, FSd) in ((T, FCL, FSL), (TC, FCC, FSC)):
        io = A.t("io", [128, TT], I32); nv = A.t("nv", [128, 1], F32); nvi = A.t("nvi", [128, 1], I32)
        k.op("pool", lambda e, io=io, TT=TT: e.iota(io[:], pattern=[[1, TT]], base=0, channel_multiplier=0), w=["io"])
        for i in range(TT // 128):
            m1 = A.t("m1", [128, TT], I32) if i == 0 else m1
            m2 = A.t("m2", [128, TT], I32) if i == 0 else m2
            fs = A.t("fs", [128, TT], BF16) if i == 0 else fs
            fc = A.t("fc", [128, TT], BF16) if i == 0 else fc
            k.op("pool", lambda e, i=i: e.iota(nvi[:], pattern=[[0, 1]], base=128 * i, channel_multiplier=1), w=["nvi"])
            k.op("dve", lambda e: e.tensor_copy(out=nv[:], in_=nvi[:]), r=["nvi"], w=["nv"])
            k.op("dve", lambda e: e.tensor_scalar(out=m1[:], in0=io[:], scalar1=nv[:, 0:1], scalar2=None, op0=ALU.mult), r=["io", "nv"], w=["m1"])
            k.op("dve", lambda e, TT=TT: e.tensor_scalar(out=m1[:], in0=m1[:], scalar1=TT - 1, scalar2=None, op0=ALU.bitwise_and), r=["m1"], w=["m1"])
            k.op("dve", lambda e, TT=TT: e.tensor_scalar(out=m2[:], in0=m1[:], scalar1=float(TT // 4), scalar2=None, op0=ALU.add), r=["m1"], w=["m2"])
            k.op("dve", lambda e, TT=TT: e.tensor_scalar(out=m2[:], in0=m2[:], scalar1=TT - 1, scalar2=None, op0=ALU.bitwise_and), r=["m2"], w=["m2"])
            k.op("act", lambda e, TT=TT: e.activation(out=fs[:], in_=m1[:], func=AF.Sin, scale=2 * np.pi / TT * (1 - 1e-6), bias=negpi[:, 0:1]), r=["m1", "negpi"], w=["fs"])
            k.op("act", lambda e, TT=TT: e.activation(out=fc[:], in_=m2[:], func=AF.Sin, scale=2 * np.pi / TT * (1 - 1e-6), bias=negpi[:, 0:1]), r=["m2", "negpi"], w=["fc"])
            k.dma("sp", lambda e, i=i, FSd=FSd: e.dma_start(out=FSd[i * 128:(i + 1) * 128, :], in_=fs[:]), r=["fs"], w=[("FS", i)], key="stfs")
            k.dma("sp", lambda e, i=i, FCd=FCd: e.dma_start(out=FCd[i * 128:(i + 1) * 128, :], in_=fc[:]), r=["fc"], w=[("FC", i)], key="stfc")
    cT = A.t("cT", [128, DK, 2], F32); cS = A.t("cS", [128, DK, 2], F32)
    ld(cT[:], condT, "cT")
    k.op("act", lambda e: e.activation(out=cS[:], in_=cT[:], func=AF.Silu), r=["cT"], w=["cS"])
    bm = A.t("bm", [2, 6 * D], F32); mo = A.t("mo", [2, 6 * D], F32)
    for l in range(2):
        for m in range(2):
            ld(bm[m:m + 1, :], b_mod[l:l + 1, :], "bm_%d" % m)
        for cs in range(12):
            wm = A.t("wm", [128, DK, 512], F32) if (l == 0 and cs == 0) else wm
            ld(wm[:], w_mod[l].rearrange("(k p) n -> p k n", p=128)[:, :, cs * 512:(cs + 1) * 512], "wm")
            pf, pk = nxF()
            for kk in range(DK):
                k.op("pe", lambda e, kk=kk, pf=pf: e.matmul(out=pf[0:2, :], lhsT=cS[:, kk, :], rhs=wm[:, kk, :], start=(kk == 0), stop=(kk == DK - 1)),
                     r=["cS", "wm"], w=[pk], sig=(kk == DK - 1))
            k.op("dve", lambda e, pf=pf, cs=cs, mo=mo, bm=bm: e.tensor_tensor(out=mo[:, cs * 512:(cs + 1) * 512], in0=pf[0:2, :], in1=bm[:, cs * 512:(cs + 1) * 512], op=ALU.add),
                 r=[pk, "bm_0", "bm_1"], w=["mo"])
        k.dma("sp", lambda e, l=l, mo=mo: e.dma_start(out=MOD[l], in_=mo[:]), r=["mo"], w=["MOD"], key="stmod")
    ect = A.t("ect", [128, 512], F32)
    for h in range(2):
        ld(ect[h * 64:(h + 1) * 64, :], ec, "ect%d" % h)
    for t in range(T // 128):
        xt = A.t("xt0", [128, D], F32) if t == 0 else xt
        er2 = A.t("er2", [2, 512], F32) if t == 0 else er2
        ld(xt[:], x_in[t * 128:(t + 1) * 128, :], "xt0")
        ld(er2[:], er[2 * t:2 * t + 2, :], "er2")
        pf, pk = nxF()
        k.op("pe", lambda e, pf=pf: e.matmul(out=pf[:], lhsT=selh_sb[:], rhs=er2[:], start=True, stop=True), r=["selh_sb", "er2"], w=[pk])
        k.op("dve", lambda e, pf=pf: e.tensor_tensor(out=xt[:, 0:512], in0=pf[:], in1=xt[:, 0:512], op=ALU.add), r=["xt0", pk], w=["xt0"])
        k.op("dve", lambda e: e.tensor_tensor(out=xt[:, 512:1024], in0=xt[:, 512:1024], in1=ect[:], op=ALU.add), r=["xt0", "ect0", "ect1"], w=["xt0"])
        k.dma("sp", lambda e, t=t: e.dma_start(out=xres[t * 128:(t + 1) * 128, :], in_=xt[:]), r=["xt0"], w=[("xres", t)], key="stx0")
    for t in range(TC // 128):
        ld(xt[:], ctx_in[t * 128:(t + 1) * 128, :], "xt0")
        k.dma("sp", lambda e, t=t: e.dma_start(out=cres[t * 128:(t + 1) * 128, :], in_=xt[:]), r=["xt0"], w=[("cres", t)], key="stx0")
    A.end()

    def bcast_tile(A, name, l, m, seg, plus_one=False, gidx=None):
        tl = A.t(name, [128, D], F32)
        mr = A.t(name + "_r", [2, D], F32)
        ld(mr[:], MOD[l][:, seg * D:(seg + 1) * D], name + "_r", r=["MOD"])
        if gidx is not None:
            gr = A.t(name + "_g", [1, D], F32)
            ld(gr[:], ng[gidx:gidx + 1, :], name + "_g")
        for hh in range(2):
            pf, pk = nxF()
            k.op("pe", lambda e, pf=pf, hh=hh: e.matmul(out=pf[:], lhsT=sel2_sb[:, m, :], rhs=mr[:, hh * 512:(hh + 1) * 512], start=True, stop=True),
                 r=["sel2_sb", name + "_r"], w=[pk])
            if gidx is None:
                k.op("act", lambda e, pf=pf, hh=hh: e.activation(out=tl[:, hh * 512:(hh + 1) * 512], in_=pf[:], func=AF.Copy), r=[pk], w=[name])
            else:
                k.op("act", lambda e, pf=pf, hh=hh: e.activation(out=tl[:, hh * 512:(hh + 1) * 512], in_=pf[:], func=AF.Identity, bias=1.0 if plus_one else 0.0), r=[pk], w=[name])
                pg, pgk = nxF()
                k.op("pe", lambda e, pg=pg, hh=hh: e.matmul(out=pg[:], lhsT=ones_sb[:], rhs=gr[:, hh * 512:(hh + 1) * 512], start=True, stop=True),
                     r=["ones_sb", name + "_g"], w=[pgk])
                k.op("dve", lambda e, pg=pg, hh=hh: e.tensor_tensor(out=tl[:, hh * 512:(hh + 1) * 512], in0=pg[:], in1=tl[:, hh * 512:(hh + 1) * 512], op=ALU.mult), r=[pgk, name], w=[name])
        return tl

    def rms_mod(A, src, srck, Gt, Gk, SHt, SHk, outt, outk, tmp):
        sq = A.t("sqj", [128, D], BF16) if "sqj" not in A.__dict__ else A.sqj
        A.sqj = sq
        k.op("act", lambda e: e.activation(out=sq[:], in_=src[:], func=AF.Square, accum_out=ssv[:, 0:1]), r=[srck], w=["sqj", "ss"])
        k.op("act", lambda e: e.activation(out=ssv[:, 1:2], in_=ssv[:, 0:1], func=AF.Sqrt, scale=1.0 / D, bias=epsb[:, 0:1]), r=["ss", "epsb"], w=["sd"])
        k.op("dve", lambda e: e.reciprocal(out=ssv[:, 2:3], in_=ssv[:, 1:2]), r=["sd"], w=["rstd"])
        k.op("dve", lambda e: e.scalar_tensor_tensor(out=tmp[:], in0=src[:], scalar=ssv[:, 2:3], in1=Gt[:], op0=ALU.mult, op1=ALU.mult), r=[srck, "rstd", Gk], w=["rmtmp"])
        k.op("dve", lambda e: e.tensor_tensor(out=outt[:], in0=tmp[:], in1=SHt[:], op=ALU.add), r=["rmtmp", SHk], w=[outk])

    epsb = psb("epsb", [128, 1], F32)
    k.op("dve", lambda e: e.memset(epsb[:], EPS), w=["epsb"])

    def mixer(l, sset, last_ctx):
        TT = T if sset == "lat" else TC
        res = xres if sset == "lat" else cres
        resk = "xres" if sset == "lat" else "cres"
        m = 0 if sset == "lat" else 1
        GS = min(512, TT); NT = TT // 128; NG = TT // GS; SPG = GS // 128
        FCd, FSd = (FCL, FSL) if sset == "lat" else (FCC, FSC)
        A = Arena(k)
        w_in_sb = A.t("w_in_sb", [128, DK, 1536], BF16)
        ld(w_in_sb[:], w_in[l].rearrange("(k p) n -> p k n", p=128), "w_in_sb", "pool")
        ld(bdb[:], bd[l].rearrange("j p n -> p j n"), "bdb", "pool")
        ld(lruv_sb[:], lruv[l].rearrange("(q p) n -> p q n", p=128), "lruv_sb")
        for dd in range(2):
            ld(lrud_sb[:, dd], lrud[l, dd].rearrange("(q p) n -> p q n", p=128), "lrud_sb%d" % dd)
        k.op("act", lambda e: e.activation(out=nls_sb[:], in_=lrud_sb[:, :, :, 2], func=AF.Exp, scale=-1.0), r=["lrud_sb0", "lrud_sb1"], w=["nls"])
        k.op("act", lambda e: e.activation(out=nls_sb[:], in_=nls_sb[:], func=AF.Ln, bias=1.0), r=["nls"], w=["nls"])
        k.op("dve", lambda e: e.tensor_scalar(out=nls_sb[:], in0=nls_sb[:], scalar1=-LRU_C, scalar2=None, op0=ALU.mult), r=["nls"], w=["nls"])
        G1 = bcast_tile(A, "G1", l, m, 1, plus_one=True, gidx=l)
        SH1 = bcast_tile(A, "SH1", l, m, 0)
        AB = None if last_ctx else A.t("AB", [128, NT, 4, 256], BF16)
        xt = [A.t("xt%d" % i, [128, D], F32) for i in range(2)]
        tmp = A.t("tmp", [128, D], F32)
        hb = [A.t("hb%d" % i, [128, D], BF16) for i in range(2)]
        hT = [A.t("hT%d" % i, [128, DK, GS], BF16) for i in range(2)]
        uf = A.t("uf", [128, GS], BF16)
        uxt = [A.t("uxt%d" % i, [128, GS], F32) for i in range(2)]
        ge = [A.t("ge%d" % i, [128, GS], F32) for i in range(3)]
        mlist = list(range(4, 8)) if last_ctx else list(range(12))
        for g in range(NG):
            hTg = hT[g % 2]; hTk = "hT%d" % (g % 2)
            for sub in range(SPG):
                t = g * SPG + sub
                xb = xt[t % 2]; xk = "xt%d" % (t % 2); hbb = hb[t % 2]; hbk = "hb%d" % (t % 2)
                ld(xb[:], res[t * 128:(t + 1) * 128, :], xk, r=[(resk, t)])
                rms_mod(A, xb, xk, G1, "G1", SH1, "SH1", hbb, hbk, tmp)
                pt, ptk = nxT()
                for kk in range(DK):
                    k.op("pe", lambda e, kk=kk, pt=pt, hbb=hbb: e.transpose(out=pt[:, kk * 128:(kk + 1) * 128], in_=hbb[:, kk * 128:(kk + 1) * 128], identity=identb[:]),
                         r=[hbk, "identb"], w=[ptk], sig=(kk == DK - 1))
                k.op("act", lambda e, pt=pt, hTg=hTg, sub=sub: e.activation(out=hTg[:, :, sub * 128:(sub + 1) * 128], in_=pt[:].rearrange("p (k n) -> p k n", k=DK), func=AF.Copy),
                     r=[ptk], w=[hTk])
            for mc in mlist:
                pf, pk = nxF()
                for kk in range(DK):
                    k.op("pe", lambda e, kk=kk, pf=pf, mc=mc, hTg=hTg: e.matmul(out=pf[:, 0:GS], lhsT=w_in_sb[:, kk, mc * 128:(mc + 1) * 128], rhs=hTg[:, kk, :], start=(kk == 0), stop=(kk == DK - 1)),
                         r=["w_in_sb", hTk], w=[pk], sig=(kk == DK - 1))
                if mc < 4:
                    k.op("act", lambda e, pf=pf: e.activation(out=uf[:], in_=pf[:, 0:GS], func=AF.Copy), r=[pk], w=["uf"])
                    for sub in range(SPG):
                        p2, p2k = nxF()
                        k.op("pe", lambda e, p2=p2, sub=sub: e.matmul(out=p2[:, 0:256], lhsT=uf[:, sub * 128:(sub + 1) * 128], rhs=cskb[:, m, :], start=True, stop=True),
                             r=["uf", "cskb"], w=[p2k])
                        k.op("dve", lambda e, p2=p2, sub=sub, mc=mc, g=g: e.tensor_copy(out=AB[:, g * SPG + sub, mc, :], in_=p2[:, 0:256]), r=[p2k], w=["AB"])
                elif mc < 8:
                    ub = uxt[mc % 2]; ubk = "uxt%d" % (mc % 2)
                    k.op("dve", lambda e, pf=pf, ub=ub: e.tensor_copy(out=ub[:], in_=pf[:, 0:GS]), r=[pk], w=[ubk])
                    k.dma("sp", lambda e, ub=ub, mc=mc, g=g: e.dma_start(out=UX[(mc - 4) * 128:(mc - 3) * 128, g * GS:(g + 1) * GS], in_=ub[:]), r=[ubk], w=[("UX", mc - 4)], key="st" + ubk)
                else:
                    k.op("act", lambda e, pf=pf: e.activation(out=ge[0][:], in_=pf[:, 0:GS], func=AF.Square), r=[pk], w=["ge0"])
                    k.op("dve", lambda e: e.tensor_scalar(out=ge[0][:], in0=ge[0][:], scalar1=0.044715, scalar2=1.0, op0=ALU.mult, op1=ALU.add), r=["ge0"], w=["ge0"])
                    k.op("dve", lambda e, pf=pf: e.tensor_tensor(out=ge[1][:], in0=pf[:, 0:GS], in1=ge[0][:], op=ALU.mult), r=[pk, "ge0"], w=["ge1"])
                    k.op("act", lambda e: e.activation(out=ge[1][:], in_=ge[1][:], func=AF.Sigmoid, scale=1.5957691216057308), r=["ge1"], w=["ge1"])
                    k.op("dve", lambda e, pf=pf: e.tensor_tensor(out=ge[2][:], in0=pf[:, 0:GS], in1=ge[1][:], op=ALU.mult), r=[pk, "ge1"], w=["ge2"])
                    k.dma("sp", lambda e, mc=mc, g=g: e.dma_start(out=GG[(mc - 8) * 128:(mc - 7) * 128, g * GS:(g + 1) * GS], in_=ge[2][:]), r=["ge2"], w=[("GG", mc - 8)], key="stge")
        if not last_ctx:
            HC = min(NT, 4)
            pan = [[A.t("pan%d_%d" % (i, j), [128, HC, GS], BF16) for j in range(2)] for i in range(2)]
            yf = A.t("yf", [128, GS], BF16)
            cnt = 0
            for j in range(NG):
                banks = [nxF() for _ in range(4)]
                for i0 in range(0, NT, HC):
                    pb = pan[cnt % 2]; pbk = ["pan%d_%d" % (cnt % 2, jj) for jj in range(2)]
                    cnt += 1
                    for jj, Fd, nm in ((0, FCd, "FC"), (1, FSd, "FS")):
                        k.dma("sp", lambda e, jj=jj, Fd=Fd, pb=pb, i0=i0, j=j: e.dma_start(out=pb[jj][:], in_=Fd[i0 * 128:(i0 + HC) * 128, j * GS:(j + 1) * GS].rearrange("(i p) n -> p i n", p=128)),
                              r=[(nm, i0 + ii) for ii in range(HC)], w=[pbk[jj]])
                    for ii in range(HC):
                        i = i0 + ii
                        for gq in range(4):
                            pf, pk = banks[gq]
                            k.op("pe", lambda e, pf=pf, i=i, ii=ii, gq=gq, pb=pb: e.matmul(out=pf[:, 0:GS], lhsT=AB[:, i, gq, 0:128], rhs=pb[0][:, ii, :], start=(i == 0), stop=False),
                                 r=["AB", pbk[0]], w=[pk], sig=False)
                            k.op("pe", lambda e, pf=pf, i=i, ii=ii, gq=gq, pb=pb: e.matmul(out=pf[:, 0:GS], lhsT=AB[:, i, gq, 128:256], rhs=pb[1][:, ii, :], start=False, stop=(i == NT - 1)),
                                 r=["AB", pbk[1]], w=[pk], sig=(gq == 3))
                for gq in range(4):
                    pf, pk = banks[gq]
                    k.op("act", lambda e, pf=pf: e.activation(out=yf[:], in_=pf[:, 0:GS], func=AF.Copy), r=[pk], w=["yf"])
                    k.dma("sp", lambda e, gq=gq, j=j: e.dma_start(out=MIXT[gq * 128:(gq + 1) * 128, j * GS:(j + 1) * GS], in_=yf[:]), r=["yf"], w=[("MIXT", gq)], key="styf")
        A.end()
        A = Arena(k)
        ux = A.t("ux", [128, TT + 3], F32); xc = A.t("xc", [128, TT], F32); xcb = A.t("xcb", [128, TT], BF16)
        a1 = A.t("a1", [128, TT], F32); a2 = A.t("a2", [128, TT], F32); a3 = A.t("a3", [128, TT], F32); yy = A.t("yy", [128, TT], F32)
        yr = A.t("yr", [128, TT], BF16)
        for q in range(4):
            k.op("pool", lambda e: e.memset(ux[:, 0:1], 0.0), w=["ux"])
            k.op("pool", lambda e: e.memset(ux[:, TT + 1:TT + 3], 0.0), w=["ux"])
            k.dma("sp", lambda e, q=q: e.dma_start(out=ux[:, 1:TT + 1], in_=UX[q * 128:(q + 1) * 128, 0:TT]), r=[("UX", q)], w=["ux"])
            k.op("dve", lambda e, q=q: e.tensor_scalar(out=xc[:], in0=ux[:, 0:TT], scalar1=lruv_sb[:, q, 0:1], scalar2=lruv_sb[:, q, 4:5], op0=ALU.mult, op1=ALU.add), r=["ux", "lruv_sb"], w=["xc"])
            for kk in range(1, 4):
                k.op("dve", lambda e, q=q, kk=kk: e.scalar_tensor_tensor(out=xc[:], in0=ux[:, kk:kk + TT], scalar=lruv_sb[:, q, kk:kk + 1], in1=xc[:], op0=ALU.mult, op1=ALU.add), r=["ux", "lruv_sb", "xc"], w=["xc"])
            k.op("act", lambda e: e.activation(out=xcb[:], in_=xc[:], func=AF.Copy), r=["xc"], w=["xcb"])
            for dd in range(2):
                for g in range(NG):
                    cs = slice(g * GS, (g + 1) * GS)
                    pf, pk = nxF()
                    k.op("pe", lambda e, pf=pf, cs=cs, dd=dd, q=q: e.matmul(out=pf[:, 0:GS], lhsT=bdb[:, (dd * 2 + 0) * 4 + q, :], rhs=xcb[:, cs], start=True, stop=True), r=["bdb", "xcb"], w=[pk])
                    k.op("act", lambda e, pf=pf, cs=cs, dd=dd, q=q: e.activation(out=a1[:, cs], in_=pf[:, 0:GS], func=AF.Sigmoid, bias=lrud_sb[:, dd, q, 0:1]), r=[pk, "lrud_sb%d" % dd], w=["a1"])
                    pf, pk = nxF()
                    k.op("pe", lambda e, pf=pf, cs=cs, dd=dd, q=q: e.matmul(out=pf[:, 0:GS], lhsT=bdb[:, (dd * 2 + 1) * 4 + q, :], rhs=xcb[:, cs], start=True, stop=True), r=["bdb", "xcb"], w=[pk])
                    k.op("act", lambda e, pf=pf, cs=cs, dd=dd, q=q: e.activation(out=a3[:, cs], in_=pf[:, 0:GS], func=AF.Sigmoid, bias=lrud_sb[:, dd, q, 1:2]), r=[pk, "lrud_sb%d" % dd], w=["a3"])
                k.op("act", lambda e, dd=dd, q=q: e.activation(out=a1[:], in_=a1[:], func=AF.Exp, scale=nls_sb[:, dd, q:q + 1]), r=["a1", "nls"], w=["a1"])
                k.op("act", lambda e: e.activation(out=a2[:], in_=a1[:], func=AF.Square), r=["a1"], w=["a2"])
                k.op("act", lambda e: e.activation(out=a2[:], in_=a2[:], func=AF.Sqrt, scale=-1.0, bias=1.0), r=["a2"], w=["a2"])
                k.op("dve", lambda e: e.tensor_tensor(out=a3[:], in0=a3[:], in1=a2[:], op=ALU.mult), r=["a3", "a2"], w=["a3"])
                k.op("dve", lambda e: e.tensor_tensor(out=a3[:], in0=a3[:], in1=xc[:], op=ALU.mult), r=["a3", "xc"], w=["a3"])
                dst = yy if dd == 0 else a2
                dk = "yy" if dd == 0 else "a2"
                init = 0.0 if sset == "ctx" else states[:, q, dd:dd + 1]
                if dd == 0:
                    k.op("dve", lambda e, init=init: e.tensor_tensor_scan(out=dst[:], data0=a1[:], data1=a3[:], initial=init, op0=ALU.mult, op1=ALU.add), r=["a1", "a3", "states"], w=[dk])
                else:
                    k.op("dve", lambda e, init=init, dst=dst: e.tensor_tensor_scan(out=dst[:, ::-1], data0=a1[:, ::-1], data1=a3[:, ::-1], initial=init, op0=ALU.mult, op1=ALU.add), r=["a1", "a3", "states"], w=[dk])
                if sset == "ctx":
                    col = TT - 1 if dd == 0 else 0
                    k.op("act", lambda e, dst=dst, col=col, q=q, dd=dd: e.activation(out=states[:, q, dd:dd + 1], in_=dst[:, col:col + 1], func=AF.Copy), r=[dk], w=["states"])
                if dd == 1:
                    k.op("dve", lambda e: e.tensor_tensor(out=yy[:], in0=yy[:], in1=a2[:], op=ALU.add), r=["yy", "a2"], w=["yy"])
            if not last_ctx:
                k.dma("sp", lambda e, q=q: e.dma_start(out=a1[:], in_=GG[q * 128:(q + 1) * 128, 0:TT]), r=[("GG", q)], w=["a1"])
                k.op("dve", lambda e: e.tensor_tensor(out=yr[:], in0=yy[:], in1=a1[:], op=ALU.mult), r=["yy", "a1"], w=["yr"])
                k.dma("sp", lambda e, q=q: e.dma_start(out=MIXT[512 + q * 128:512 + (q + 1) * 128, 0:TT], in_=yr[:]), r=["yr"], w=[("MIXT", 4 + q)], key="styr")
        A.end()
        if last_ctx:
            return
        A = Arena(k)
        w_out_sb = A.t("w_out_sb", [128, DK, D], BF16)
        ld(w_out_sb[:], w_out[l].rearrange("(k p) n -> p k n", p=128), "w_out_sb", "pool")
        ld(wr_sb[:], w_router[l].rearrange("(k p) n -> p k n", p=128), "wr_sb")
        GT1 = bcast_tile(A, "GT1", l, m, 2)
        G2 = bcast_tile(A, "G2", l, m, 4, plus_one=True, gidx=2 + l)
        SH2 = bcast_tile(A, "SH2", l, m, 3)
        mx = [A.t("mx%d" % i, [128, DK, GS], BF16) for i in range(2)]
        xt = [A.t("xt%d" % i, [128, D], F32) for i in range(2)]
        tmp = A.t("tmp", [128, D], F32); xn = A.t("xn", [128, D], F32); h2 = A.t("h2", [128, D], F32)
        h2b = A.t("h2b", [128, D], BF16); h2T = A.t("h2T", [128, DK, 128], F32)
        lg = A.t("lg", [128, NE], F32); aff = A.t("aff", [128, NE], F32)
        AFFT = A.t("AFFT", [NE, TT], F32)
        hoff = 0 if sset == "lat" else T
        for g in range(NG):
            mxg = mx[g % 2]; mxk = "mx%d" % (g % 2)
            k.dma("sp", lambda e, mxg=mxg, g=g: e.dma_start(out=mxg[:], in_=MIXT[:, g * GS:(g + 1) * GS].rearrange("(k p) n -> p k n", p=128)), r=[("MIXT", i) for i in range(8)], w=[mxk])
            for sub in range(SPG):
                t = g * SPG + sub
                xb = xt[t % 2]; xk = "xt%d" % (t % 2)
                ld(xb[:], res[t * 128:(t + 1) * 128, :], xk, r=[(resk, t)])
                for hh in range(2):
                    pf, pk = nxF()
                    for kk in range(DK):
                        k.op("pe", lambda e, pf=pf, kk=kk, hh=hh, mxg=mxg, sub=sub: e.matmul(out=pf[:], lhsT=mxg[:, kk, sub * 128:(sub + 1) * 128], rhs=w_out_sb[:, kk, hh * 512:(hh + 1) * 512], start=(kk == 0), stop=(kk == DK - 1)),
                             r=[mxk, "w_out_sb"], w=[pk], sig=(kk == DK - 1))
                    k.op("dve", lambda e, pf=pf, hh=hh: e.tensor_tensor(out=tmp[:, hh * 512:(hh + 1) * 512], in0=pf[:], in1=GT1[:, hh * 512:(hh + 1) * 512], op=ALU.mult), r=[pk, "GT1"], w=["tmpo"])
                k.op("pool", lambda e, xb=xb: e.tensor_tensor(out=xn[:], in0=xb[:], in1=tmp[:], op=ALU.add), r=[xk, "tmpo"], w=["xn"])
                k.dma("sp", lambda e, t=t: e.dma_start(out=res[t * 128:(t + 1) * 128, :], in_=xn[:]), r=["xn"], w=[(resk, t)], key="stxn")
                rms_mod(A, xn, "xn", G2, "G2", SH2, "SH2", h2, "h2", tmp)
                k.op("act", lambda e: e.activation(out=h2b[:], in_=h2[:], func=AF.Copy), r=["h2"], w=["h2b"])
                k.dma("sp", lambda e, t=t: e.dma_start(out=HX2[hoff + t * 128:hoff + (t + 1) * 128, :], in_=h2b[:]), r=["h2b"], w=[("HX2", sset, t)], key="sth2b")
                for q2 in range(2):
                    pf, pk = nxF()
                    for kk in range(4):
                        k.op("pe", lambda e, pf=pf, kk=kk, q2=q2: e.transpose(out=pf[:, kk * 128:(kk + 1) * 128], in_=h2[:, (q2 * 4 + kk) * 128:(q2 * 4 + kk + 1) * 128], identity=identf[:]),
                             r=["h2", "identf"], w=[pk], sig=(kk == 3))
                    k.op("act", lambda e, pf=pf, q2=q2: e.activation(out=h2T[:, q2 * 4:(q2 + 1) * 4, :], in_=pf[:].rearrange("p (k n) -> p k n", k=4), func=AF.Copy), r=[pk], w=["h2T"])
                pf, pk = nxF()
                for kk in range(DK):
                    k.op("pe", lambda e, pf=pf, kk=kk: e.matmul(out=pf[:, 0:NE], lhsT=h2T[:, kk, :], rhs=wr_sb[:, kk, :], start=(kk == 0), stop=(kk == DK - 1)), r=["h2T", "wr_sb"], w=[pk], sig=(kk == DK - 1))
                k.op("dve", lambda e, pf=pf: e.tensor_copy(out=lg[:], in_=pf[:, 0:NE]), r=[pk], w=["lg"])
                k.op("dve", lambda e: e.tensor_reduce(out=ssv[:, 3:4], in_=lg[:], axis=AX.X, op=ALU.max, negate=True), r=["lg"], w=["nmx"])
                k.op("act", lambda e: e.activation(out=aff[:], in_=lg[:], func=AF.Exp, bias=ssv[:, 3:4], accum_out=ssv[:, 4:5]), r=["lg", "nmx"], w=["aff", "esum"])
                k.op("dve", lambda e: e.reciprocal(out=ssv[:, 5:6], in_=ssv[:, 4:5]), r=["esum"], w=["resum"])
                k.op("dve", lambda e: e.tensor_scalar(out=aff[:], in0=aff[:], scalar1=ssv[:, 5:6], scalar2=None, op0=ALU.mult), r=["aff", "resum"], w=["aff"])
                pf, pk = nxF()
                k.op("pe", lambda e, pf=pf: e.transpose(out=pf[0:NE, 0:128], in_=aff[:, 0:NE], identity=identf[:]), r=["aff", "identf"], w=[pk])
                k.op("act", lambda e, pf=pf, t=t: e.activation(out=AFFT[:, t * 128:(t + 1) * 128], in_=pf[0:NE, 0:128], func=AF.Copy), r=[pk], w=["AFFT"])
        C = CL if sset == "lat" else CC
        coff = 0 if sset == "lat" else 2 * CL
        for rnd in range(C // 8):
            sl = slice(rnd * 8, (rnd + 1) * 8)
            k.op("dve", lambda e, sl=sl: e.max(out=VAL[sset][:, sl], in_=AFFT[:]), r=["AFFT"], w=["VAL" + sset])
            k.op("dve", lambda e, sl=sl: e.max_index(out=IDXU[sset][:, sl], in_max=VAL[sset][:, sl], in_values=AFFT[:]), r=["AFFT", "VAL" + sset], w=["IDXU" + sset])
            k.op("dve", lambda e, sl=sl: e.match_replace(out=AFFT[:], in_to_replace=VAL[sset][:, sl], in_values=AFFT[:], imm_value=-1.0), r=["AFFT", "VAL" + sset], w=["AFFT"])
        k.op("dve", lambda e: e.tensor_copy(out=IDXF[sset][:, 0:C], in_=IDXU[sset][:, 0:C]), r=["IDXU" + sset], w=["IDXF" + sset])
        k.dma("sp", lambda e: e.dma_start(out=TAB[:, coff:coff + C], in_=IDXF[sset][:, 0:C]), r=["IDXF" + sset], w=[("TAB", sset, 0)], key="sttab")
        k.dma("sp", lambda e: e.dma_start(out=TAB[:, coff + C:coff + 2 * C], in_=VAL[sset][:, 0:C]), r=["VAL" + sset], w=[("TAB", sset, 1)], key="sttab")
        for ch in range((C + 127) // 128):
            rows = min(128, C - ch * 128)
            pf, pk = nxF()
            k.op("pe", lambda e, pf=pf, ch=ch, rows=rows: e.transpose(out=pf[0:rows, 0:NE], in_=IDXF[sset][:, ch * 128:ch * 128 + rows], identity=identf[0:NE, 0:NE]), r=["IDXF" + sset, "identf"], w=[pk])
            k.op("dve", lambda e, pf=pf, ch=ch, rows=rows: e.tensor_copy(out=OWNI[sset][0:rows, ch, :], in_=pf[0:rows, 0:NE]), r=[pk], w=["OWNI" + sset])
            pf, pk = nxF()
            k.op("pe", lambda e, pf=pf, ch=ch, rows=rows: e.transpose(out=pf[0:rows, 0:NE], in_=VAL[sset][:, ch * 128:ch * 128 + rows], identity=identf[0:NE, 0:NE]), r=["VAL" + sset, "identf"], w=[pk])
            k.op("dve", lambda e, pf=pf, ch=ch, rows=rows: e.tensor_copy(out=OWNG[sset][0:rows, ch, :], in_=pf[0:rows, 0:NE]), r=[pk], w=["OWNG" + sset])
        A.end()

    def moe(l):
        sets = [("lat", T, CL, 0, 0)] + ([("ctx", TC, CC, T, 2 * CL)] if l == 0 else [])
        nte = B * CL + (B * CC if l == 0 else 0)
        A = Arena(k)
        k.dma("pool", lambda e: e.collective_compute("AllGather", ALU.bypass, replica_groups=[list(range(NCORES))], ins=[HX2.opt()], outs=[HX2A.opt()]),
              r=[("HX2", "lat", t) for t in range(T // 128)] + [("HX2", "ctx", t) for t in range(TC // 128)], w=["HX2A"], key="cc", inc=1)
        k.dma("pool", lambda e: e.collective_compute("AllGather", ALU.bypass, replica_groups=[list(range(NCORES))], ins=[TAB.opt()], outs=[TABA.opt()]),
              r=[("TAB", s, i) for s in ("lat", "ctx") for i in range(2)], w=["TABA"], key="cc", inc=1)
        tiles = []
        col = 0
        for (s, TT, C, hoff, coff) in sets:
            for b in range(B):
                for ch in range((C + 127) // 128):
                    rows = min(128, C - ch * 128)
                    tiles.append((s, b, ch, rows, col, hoff, coff, C))
                    col += rows
        assert col == nte
        NRT = (nte + 127) // 128
        ig = A.t("ig", [128, len(tiles), 4], F32); igi = A.t("igi", [128, len(tiles), 2], I32)
        tabb = [A.t("tabb%d" % i, [NE, W], F32) for i in range(2)]
        lastb = -1
        for ti, (s, b, ch, rows, col0, hoff, coff, C) in enumerate(tiles):
            tb = tabb[b % 2]; tbk = "tabb%d" % (b % 2)
            if b != lastb:
                ld(tb[:], TABA[(2 * b) * NE:(2 * b + 1) * NE, :], tbk, r=["TABA"])
                lastb = b
            pf, pk = nxF()
            k.op("pe", lambda e, pf=pf, tb=tb, rows=rows, c0=coff + ch * 128: e.matmul(out=pf[0:rows, 0:2], lhsT=tb[:, c0:c0 + rows], rhs=sel_sb[:], start=True, stop=True), r=[tbk, "sel_sb"], w=[pk])
            k.op("dve", lambda e, pf=pf, ti=ti, rows=rows, gb=float((2 * b) * RT + hoff): e.tensor_scalar(out=ig[0:rows, ti, 0:2], in0=pf[0:rows, 0:2], scalar1=gb, scalar2=None, op0=ALU.add), r=[pk], w=["ig"])
            k.op("dve", lambda e, ti=ti, rows=rows: e.tensor_copy(out=igi[0:rows, ti, :], in_=ig[0:rows, ti, 0:2]), r=["ig"], w=["igi"])
        XgT = A.t("XgT", [128, DK, nte], BF16)
        xg = [A.t("xg%d" % i, [128, D], BF16) for i in range(2)]
        yacc = A.t("yacc", [128, NRT, D], F32)
        wgb = [A.t("wgb%d" % i, [128, DK, 256], BF16) for i in range(2)]
        wub = [A.t("wub%d" % i, [128, DK, 256], BF16) for i in range(2)]
        wdb = [A.t("wdb%d" % i, [128, 2, D], BF16) for i in range(2)]
        hid = A.t("hid", [128, 2, nte], BF16)
        sg = A.t("sg", [128, 512], F32)
        yo = [A.t("yo%d" % i, [128, D], F32) for i in range(2)]
        ntl = [(n0, min(512, nte - n0)) for n0 in range(0, nte, 512)]
        wcnt = 0
        for el in range(2):
            for ti, (s, b, ch, rows, col0, hoff, coff, C) in enumerate(tiles):
                xb = xg[ti % 2]; xk = "xg%d" % (ti % 2)
                base = (2 * b) * RT + hoff
                TT = T if s == "lat" else TC
                k.dma("pool", lambda e, xb=xb, rows=rows, base=base, TT=TT, ti=ti, el=el: e.indirect_dma_start(out=xb[0:rows, :], out_offset=None, in_=HX2A,
                      in_offset=bass.IndirectOffsetOnAxis(ap=igi[0:rows, ti, el:el + 1], axis=0)), r=["igi", "HX2A"], w=[xk])
                pt, ptk = nxT()
                for kk in range(DK):
                    k.op("pe", lambda e, pt=pt, kk=kk, xb=xb, rows=rows: e.transpose(out=pt[:, kk * 128:kk * 128 + rows], in_=xb[0:rows, kk * 128:(kk + 1) * 128], identity=identb[0:rows, 0:rows]),
                         r=[xk, "identb"], w=[ptk], sig=(kk == DK - 1))
                k.op("act", lambda e, pt=pt, rows=rows, col0=col0: e.activation(out=XgT[:, :, col0:col0 + rows], in_=pt[:].rearrange("p (k n) -> p k n", k=DK)[:, :, 0:rows], func=AF.Copy), r=[ptk], w=["XgT"])
            for fb in range(NFB):
                wi = wcnt % 2; wcnt += 1
                f0 = fb * 256
                k.dma("pool", lambda e, wi=wi, el=el, f0=f0: e.dma_start(out=wgb[wi][:], in_=wg[l, el].rearrange("(k p) f -> p k f", p=128)[:, :, f0:f0 + 256]), w=["wgb%d" % wi])
                k.dma("pool", lambda e, wi=wi, el=el, f0=f0: e.dma_start(out=wub[wi][:], in_=wu[l, el].rearrange("(k p) f -> p k f", p=128)[:, :, f0:f0 + 256]), w=["wub%d" % wi])
                k.dma("pool", lambda e, wi=wi, el=el, f0=f0: e.dma_start(out=wdb[wi][:], in_=wd[l, el][f0:f0 + 256, :].rearrange("(c p) n -> p c n", p=128)), w=["wdb%d" % wi])
                for (n0, nn) in ntl:
                    for fc in range(2):
                        pG, pGk = nxF(); pU, pUk = nxF()
                        for kk in range(DK):
                            k.op("pe", lambda e, pG=pG, kk=kk, fc=fc, n0=n0, nn=nn, wi=wi: e.matmul(out=pG[:, 0:nn], lhsT=wgb[wi][:, kk, fc * 128:(fc + 1) * 128], rhs=XgT[:, kk, n0:n0 + nn], start=(kk == 0), stop=(kk == DK - 1)),
                                 r=["wgb%d" % wi, "XgT"], w=[pGk], sig=(kk == DK - 1))
                        for kk in range(DK):
                            k.op("pe", lambda e, pU=pU, kk=kk, fc=fc, n0=n0, nn=nn, wi=wi: e.matmul(out=pU[:, 0:nn], lhsT=wub[wi][:, kk, fc * 128:(fc + 1) * 128], rhs=XgT[:, kk, n0:n0 + nn], start=(kk == 0), stop=(kk == DK - 1)),
                                 r=["wub%d" % wi, "XgT"], w=[pUk], sig=(kk == DK - 1))
                        k.op("act", lambda e, pG=pG, nn=nn: e.activation(out=sg[:, 0:nn], in_=pG[:, 0:nn], func=AF.Silu), r=[pGk], w=["sg"])
                        k.op("dve", lambda e, pU=pU, nn=nn, n0=n0, fc=fc: e.tensor_tensor(out=hid[:, fc, n0:n0 + nn], in0=pU[:, 0:nn], in1=sg[:, 0:nn], op=ALU.mult), r=[pUk, "sg"], w=["hid"])
                for rt in range(NRT):
                    rows = min(128, nte - rt * 128)
                    for hh in range(2):
                        pY, pYk = nxF()
                        for fc in range(2):
                            k.op("pe", lambda e, pY=pY, fc=fc, rt=rt, rows=rows, hh=hh, wi=wi: e.matmul(out=pY[0:rows, :], lhsT=hid[:, fc, rt * 128:rt * 128 + rows], rhs=wdb[wi][:, fc, hh * 512:(hh + 1) * 512], start=(fc == 0), stop=(fc == 1)),
                                 r=["hid", "wdb%d" % wi], w=[pYk], sig=(fc == 1))
                        if fb == 0:
                            k.op("dve", lambda e, pY=pY, rt=rt, rows=rows, hh=hh: e.tensor_copy(out=yacc[0:rows, rt, hh * 512:(hh + 1) * 512], in_=pY[0:rows, :]), r=[pYk], w=[("yacc", rt)])
                        else:
                            k.op("dve", lambda e, pY=pY, rt=rt, rows=rows, hh=hh: e.tensor_tensor(out=yacc[0:rows, rt, hh * 512:(hh + 1) * 512], in0=pY[0:rows, :], in1=yacc[0:rows, rt, hh * 512:(hh + 1) * 512], op=ALU.add), r=[pYk, ("yacc", rt)], w=[("yacc", rt)])
            for ti, (s, b, ch, rows, col0, hoff, coff, C) in enumerate(tiles):
                rt, r0 = col0 // 128, col0 % 128
                yb = yo[ti % 2]; ybk = "yo%d" % (ti % 2)
                k.dma("sp", lambda e, rt=rt, r0=r0, rows=rows, col0=col0, el=el: e.dma_start(out=Y[el * NTE + col0:el * NTE + col0 + rows, :], in_=yacc[r0:r0 + rows, rt, :]), r=[("yacc", rt)], w=[("Y", el, col0)], key="styraw")
        k.dma("pool", lambda e: e.collective_compute("AllGather", ALU.bypass, replica_groups=[list(range(NCORES))], ins=[Y.opt()], outs=[YA.opt()]),
              r=[("Y", el, t[4]) for el in range(2) for t in tiles], w=["YA"], key="cc", inc=1)
        GT2 = {}
        for (s, TT, C, hoff, coff) in sets:
            GT2[s] = bcast_tile(A, "GT2" + s, l, 0 if s == "lat" else 1, 5)
        fi = A.t("fi", [128, 1], I32); fi2 = A.t("fi2", [128, 1], I32)
        yg = [A.t("yg%d" % i, [128, D], F32) for i in range(2)]
        yg2 = [A.t("yg2%d" % i, [128, D], F32) for i in range(2)]
        cnt = 0
        for rp in range(NCORES):
            for el in range(2):
                ex = 2 * rp + el
                for si, (s, TT, C, hoff, coff) in enumerate(sets):
                    res = xres if s == "lat" else cres
                    resk = "xres" if s == "lat" else "cres"
                    sbase = 0 if s == "lat" else B * CL
                    for ch in range((C + 127) // 128):
                        rows = min(128, C - ch * 128)
                        yb = yg[cnt % 2]; ybk = "yg%d" % (cnt % 2); y2 = yg2[cnt % 2]; y2k = "yg2%d" % (cnt % 2); cnt += 1
                        k.op("pool", lambda e, rp=rp, el=el, sbase=sbase, ch=ch: e.iota(fi[:], pattern=[[0, 1]], base=(rp * 2 + el) * NTE + sbase + ch * 128, channel_multiplier=1), w=["fi"])
                        k.op("dve", lambda e, si=si: e.tensor_scalar(out=fi2[:], in0=fi[:], scalar1=ybase_sb[:, si:si + 1], scalar2=None, op0=ALU.add), r=["fi", "ybase_sb"], w=["fi2"])
                        k.dma("pool", lambda e, yb=yb, rows=rows: e.indirect_dma_start(out=yb[0:rows, :], out_offset=None, in_=YA, in_offset=bass.IndirectOffsetOnAxis(ap=fi2[0:rows, 0:1], axis=0)), r=["fi2", "YA"], w=[ybk])
                        k.op("dve", lambda e, yb=yb, y2=y2, rows=rows, s=s: e.tensor_tensor(out=y2[0:rows, :], in0=yb[0:rows, :], in1=GT2[s][0:rows, :], op=ALU.mult), r=[ybk, "GT2" + s], w=[y2k])
                        k.op("dve", lambda e, y2=y2, rows=rows, s=s, ch=ch, ex=ex: e.tensor_scalar(out=y2[0:rows, :], in0=y2[0:rows, :], scalar1=OWNG[s][0:rows, ch, ex:ex + 1], scalar2=None, op0=ALU.mult), r=[y2k, "OWNG" + s], w=[y2k])
                        k.dma("pool", lambda e, y2=y2, rows=rows, res=res, s=s, ch=ch, ex=ex: e.indirect_dma_start(out=res, out_offset=bass.IndirectOffsetOnAxis(ap=OWNI[s][0:rows, ch, ex:ex + 1], axis=0), in_=y2[0:rows, :], in_offset=None, compute_op=ALU.add),
                              r=[y2k, "OWNI" + s] + [(resk, t) for t in range(TT // 128)], w=[(resk, t) for t in range(TT // 128)], key="scat")
        A.end()

    for l in range(2):
        last = (l == 1)
        mixer(l, "ctx", last_ctx=last)
        mixer(l, "lat", last_ctx=False)
        moe(l)
    A = Arena(k)
    gf = A.t("gf", [1, D], F32); GFt = A.t("GFt", [128, D], F32)
    ld(gf[:], ng[4:5, :], "gf")
    for hh in range(2):
        pf, pk = nxF()
        k.op("pe", lambda e, pf=pf, hh=hh: e.matmul(out=pf[:], lhsT=ones_sb[:], rhs=gf[:, hh * 512:(hh + 1) * 512], start=True, stop=True), r=["ones_sb", "gf"], w=[pk])
        k.op("act", lambda e, pf=pf, hh=hh: e.activation(out=GFt[:, hh * 512:(hh + 1) * 512], in_=pf[:], func=AF.Copy), r=[pk], w=["GFt"])
    xt = [A.t("xt%d" % i, [128, D], F32) for i in range(2)]
    ot = [A.t("ot%d" % i, [128, D], F32) for i in range(2)]
    sq = A.t("sqf", [128, D], BF16)
    for t in range(T // 128):
        xb = xt[t % 2]; xk = "xt%d" % (t % 2); ob = ot[t % 2]; ok = "ot%d" % (t % 2)
        ld(xb[:], xres[t * 128:(t + 1) * 128, :], xk, r=[("xres", t)])
        k.op("act", lambda e, xb=xb: e.activation(out=sq[:], in_=xb[:], func=AF.Square, accum_out=ssv[:, 0:1]), r=[xk], w=["sqf", "ss"])
        k.op("act", lambda e: e.activation(out=ssv[:, 1:2], in_=ssv[:, 0:1], func=AF.Sqrt, scale=1.0 / D, bias=epsb[:, 0:1]), r=["ss", "epsb"], w=["sd"])
        k.op("dve", lambda e: e.reciprocal(out=ssv[:, 2:3], in_=ssv[:, 1:2]), r=["sd"], w=["rstd"])
        k.op("dve", lambda e, xb=xb, ob=ob: e.scalar_tensor_tensor(out=ob[:], in0=xb[:], scalar=ssv[:, 2:3], in1=GFt[:], op0=ALU.mult, op1=ALU.mult), r=[xk, "rstd", "GFt"], w=[ok])
        k.dma("sp", lambda e, ob=ob, t=t: e.dma_start(out=out[t * 128:(t + 1) * 128, :], in_=ob[:]), r=[ok], w=[("out", t)], key="st" + ok)
    A.end()
    P.close()
    k.st.close()
    return nc


def _host_inputs(inp, cfg):
    T, TC, DFF = cfg["T"], cfg["TC"], cfg["DFF"]
    f = lambda a: np.ascontiguousarray(np.asarray(a, dtype=np.float32))
    x, c, ctx, c_ctx = f(inp["x"]), f(inp["c"]), f(inp["ctx"]), f(inp["c_ctx"])
    CL, CC = 2 * T // NE, 2 * TC // NE
    rows = T // 64
    quarter = D // 4
    omega = 1.0 / (10000.0 ** (np.arange(quarter, dtype=np.float32) / quarter))
    enc = lambda p: np.concatenate([np.sin(p[:, None] * omega), np.cos(p[:, None] * omega)], axis=-1).astype(np.float32)
    er = enc(np.arange(rows, dtype=np.float32)); ec = enc(np.arange(64, dtype=np.float32))
    kk = np.arange(128)
    ang = 2 * np.pi * np.outer(kk, kk) / 128.0
    csk_by_T = {}
    ident = np.eye(128, dtype=np.float32)
    sel2 = np.zeros((2, 2, 128), np.float32); sel2[0, 0] = 1; sel2[1, 1] = 1
    ones1 = np.ones((1, 128), np.float32)
    lruv = np.zeros((2, 512, 8), np.float32)
    lruv[:, :, 0:4] = np.transpose(f(inp["conv_w"]), (0, 2, 1)); lruv[:, :, 4] = f(inp["conv_b"])
    lrud = np.zeros((2, 2, 512, 4), np.float32)
    lrud[..., 0] = f(inp["lru_ba"]); lrud[..., 1] = f(inp["lru_bi"]); lrud[..., 2] = f(inp["lru_lambda"])
    bd = np.zeros((2, 16, 128, 128), np.float32)
    wa, wi = f(inp["lru_wa"]), f(inp["lru_wi"])
    for l in range(2):
        for dd in range(2):
            for wh, wsrc in enumerate((wa, wi)):
                for q in range(4):
                    j = (dd * 2 + wh) * 4 + q
                    bd[l, j, 0:64, 0:64] = wsrc[l, dd, 2 * q]; bd[l, j, 64:128, 64:128] = wsrc[l, dd, 2 * q + 1]
    ng = np.stack([f(inp["norm1_g"])[0], f(inp["norm1_g"])[1], f(inp["norm2_g"])[0], f(inp["norm2_g"])[1], f(inp["final_g"])])
    maps = []
    for r in range(NCORES):
        b = r // 2
        condT = np.stack([c[b].reshape(DK, 128).T, c_ctx.reshape(DK, 128).T], axis=-1)
        sel = np.zeros((NE, 2), np.float32); sel[2 * r, 0] = 1; sel[2 * r + 1, 1] = 1
        ybase = np.zeros((128, 2), np.float32); ybase[:, 0] = b * CL; ybase[:, 1] = b * CC
        m = {"x": x[b], "ctx": ctx[b], "condT": np.ascontiguousarray(condT), "w_mod": f(inp["w_mod"]), "b_mod": f(inp["b_mod"]), "ng": ng,
             "w_in": f(inp["w_in"]), "w_out": f(inp["w_out"]), "lruv": lruv, "lrud": lrud, "bd": bd, "w_router": f(inp["w_router"]),
             "wg": f(inp["w_gate"])[:, 2 * r:2 * r + 2], "wu": f(inp["w_up"])[:, 2 * r:2 * r + 2], "wd": f(inp["w_down"])[:, 2 * r:2 * r + 2],
             "sel": sel, "ybase": ybase, "ident": ident, "er": er, "ec": ec, "sel2": sel2, "ones1": ones1}
        cs = []
        for TT in (T, TC):
            nrm = 1.0 / np.sqrt(TT * 128.0)
            cs.append(np.concatenate([-nrm * np.cos(ang), nrm * np.sin(ang)], axis=1).astype(np.float32))
        m["csk"] = np.stack(cs)
        selh = np.zeros((2, 128), np.float32); selh[0, :64] = 1; selh[1, 64:] = 1
        m["selh"] = selh
        maps.append({kk2: np.ascontiguousarray(v) for kk2, v in m.items()})
    return maps


_NC_CACHE = {}


def kernel(**inp):
    T = int(np.asarray(inp["x"]).shape[1]); TC = int(np.asarray(inp["ctx"]).shape[1]); DFF = int(np.asarray(inp["w_gate"]).shape[-1])
    cfg = {"T": T, "TC": TC, "DFF": DFF}
    maps = _host_inputs(inp, cfg)
    key = (T, TC, DFF)
    if key not in _NC_CACHE:
        _NC_CACHE[key] = build(cfg)
    res = run_bass_kernel_spmd(_NC_CACHE[key], maps, core_ids=list(range(NCORES)))
    return np.stack([np.asarray(res.results[2 * b]["out"]) for b in range(B)]).astype(np.float32)
```
